# Optimizing a Trainium2 kernel written in Bass

```python
import jax, jax.numpy as jnp
from jax import lax
import numpy as np

D_MODEL = 2048
BATCH = 4
SEQ = 2048
DEPTH = 1

N_HEADS = 16
HEAD_DIM = D_MODEL // N_HEADS
ATTN_WIDTH = N_HEADS * HEAD_DIM
CONV_WIDTH = D_MODEL
CONV_KERNEL = 3
MOBA_BLOCK = 256
MOBA_TOPK = 3
Q_CHUNK = 16
D_FF = ((8 * D_MODEL // 3 + 255) // 256) * 256
RMS_EPS = 1e-6
IN_WIDTH = 3 * ATTN_WIDTH + 3 * CONV_WIDTH + 2 * D_MODEL

kernel_name = 'macaron_hybrid_moba_shortconv_alibi'


def rms_norm(x, g):
    xf = x.astype(jnp.float32)
    y = xf * lax.rsqrt(jnp.mean(xf * xf, axis=-1, keepdims=True) + RMS_EPS)
    return (y * g.astype(jnp.float32)).astype(x.dtype)


def swiglu(h, w_gate, w_up, w_down):
    return (jax.nn.silu(h @ w_gate) * (h @ w_up)) @ w_down


def alibi_slopes():
    return jnp.exp2(-8.0 * jnp.arange(1, N_HEADS + 1, dtype=jnp.float32) / N_HEADS)


def moba_attention(q, k, v):
    bsz, s, h, hd = q.shape
    nb = -(-s // MOBA_BLOCK)
    s_pad = nb * MOBA_BLOCK
    pad = ((0, 0), (0, s_pad - s), (0, 0), (0, 0))
    q, k, v = [jnp.pad(a, pad).transpose(0, 2, 1, 3) for a in (q, k, v)]
    kb = k.reshape(bsz, h, nb, MOBA_BLOCK, hd)
    vb = v.reshape(bsz, h, nb, MOBA_BLOCK, hd)
    slopes = alibi_slopes()
    scale = HEAD_DIM ** -0.5

    kmean = jnp.mean(kb.astype(jnp.float32), axis=3)
    gate = jnp.einsum('bhqd,bhnd->bhqn', q.astype(jnp.float32), kmean)
    qblk = jnp.arange(s_pad) // MOBA_BLOCK
    past = jnp.arange(nb)[None, :] < qblk[:, None]
    gate = jnp.where(past, gate, -jnp.inf)
    n_sel = min(MOBA_TOPK, nb)
    _, sel = lax.top_k(gate, n_sel)

    n_chunks = s_pad // Q_CHUNK
    q_chunks = jnp.moveaxis(q.reshape(bsz, h, n_chunks, Q_CHUNK, hd), 2, 0)
    sel_chunks = jnp.moveaxis(sel.reshape(bsz, h, n_chunks, Q_CHUNK, n_sel), 2, 0)
    starts = jnp.arange(n_chunks, dtype=jnp.int32) * Q_CHUNK
    bi = jnp.arange(bsz)[:, None, None, None]
    hi = jnp.arange(h)[None, :, None, None]
    offs = jnp.arange(MOBA_BLOCK)

    def one_chunk(args):
        qc, selc, start = args
        tq = start + jnp.arange(Q_CHUNK)
        own = start // MOBA_BLOCK
        kg = kb[bi, hi, selc]
        vg = vb[bi, hi, selc]
        ko = lax.dynamic_index_in_dim(kb, own, axis=2, keepdims=False)
        vo = lax.dynamic_index_in_dim(vb, own, axis=2, keepdims=False)
        kpos_sel = selc[..., None] * MOBA_BLOCK + offs
        dist_sel = (tq[:, None, None] - kpos_sel).astype(jnp.float32)
        s_sel = jnp.einsum('bhqd,bhqnjd->bhqnj', qc, kg,
                           preferred_element_type=jnp.float32) * scale
        s_sel = s_sel - slopes[:, None, None, None] * dist_sel
        s_sel = jnp.where((selc < own)[..., None], s_sel, -jnp.inf)
        kpos_own = own * MOBA_BLOCK + offs
        dist_own = (tq[:, None] - kpos_own[None, :]).astype(jnp.float32)
        s_own = jnp.einsum('bhqd,bhjd->bhqj', qc, ko,
                           preferred_element_type=jnp.float32) * scale
        s_own = s_own - slopes[:, None, None] * dist_own
        s_own = jnp.where(dist_own >= 0, s_own, -jnp.inf)
        scores = jnp.concatenate(
            [s_sel.reshape(bsz, h, Q_CHUNK, n_sel * MOBA_BLOCK), s_own], axis=-1)
        p = jax.nn.softmax(scores, axis=-1).astype(vg.dtype)
        p_sel = p[..., :n_sel * MOBA_BLOCK].reshape(bsz, h, Q_CHUNK, n_sel, MOBA_BLOCK)
        p_own = p[..., n_sel * MOBA_BLOCK:]
        return (jnp.einsum('bhqnj,bhqnjd->bhqd', p_sel, vg)
                + jnp.einsum('bhqj,bhjd->bhqd', p_own, vo))

    out = lax.map(one_chunk, (q_chunks, sel_chunks, starts))
    out = jnp.moveaxis(out, 0, 2).reshape(bsz, h, s_pad, hd)
    out = out.transpose(0, 2, 1, 3)[:, :s]
    return out.reshape(bsz, s, h * hd)


def short_conv(u, conv_w):
    rhs = conv_w[:, None, :]
    return lax.conv_general_dilated(
        u, rhs, window_strides=(1,), padding=[(CONV_KERNEL - 1, 0)],
        dimension_numbers=('NWC', 'WIO', 'NWC'), feature_group_count=u.shape[-1])


def hybrid_mixer(h, w_in, b_gate, conv_w, w_attn_out, w_conv_out, w_out):
    bsz, s, _ = h.shape
    z = h @ w_in
    cuts = np.cumsum([ATTN_WIDTH] * 3 + [CONV_WIDTH] * 3).tolist()
    q, k, v, gb, gc, xt, g_logits = jnp.split(z, cuts, axis=-1)
    g_logits = (g_logits + b_gate).astype(jnp.float32)
    g_attn = jax.nn.sigmoid(g_logits[..., :D_MODEL]).astype(h.dtype)
    g_conv = jax.nn.sigmoid(g_logits[..., D_MODEL:]).astype(h.dtype)
    shp = (bsz, s, N_HEADS, HEAD_DIM)
    y_attn = moba_attention(q.reshape(shp), k.reshape(shp), v.reshape(shp)) @ w_attn_out
    y_conv = (gb * short_conv(gc * xt, conv_w)) @ w_conv_out
    return (g_attn * y_attn + g_conv * y_conv) @ w_out


def setup_inputs(seed: int = 0) -> dict:
    key = jax.random.key(seed)
    ks = jax.random.split(key, 20)

    def nrm(k, shape, fan_in):
        return jax.random.normal(k, shape, jnp.float32) * (fan_in ** -0.5)

    def gain(k):
        return 1.0 + 0.01 * jax.random.normal(k, (DEPTH, D_MODEL), jnp.float32)

    return {
        'x': jax.random.normal(ks[0], (BATCH, SEQ, D_MODEL), jnp.float32),
        'ffn1_norm': gain(ks[1]),
        'ffn1_w_gate': nrm(ks[2], (DEPTH, D_MODEL, D_FF), D_MODEL),
        'ffn1_w_up': nrm(ks[3], (DEPTH, D_MODEL, D_FF), D_MODEL),
        'ffn1_w_down': nrm(ks[4], (DEPTH, D_FF, D_MODEL), D_FF),
        'mix_norm': gain(ks[5]),
        'w_in': nrm(ks[6], (DEPTH, D_MODEL, IN_WIDTH), D_MODEL),
        'b_gate': 0.01 * jax.random.normal(ks[7], (DEPTH, 2 * D_MODEL), jnp.float32),
        'conv_w': nrm(ks[8], (DEPTH, CONV_KERNEL, CONV_WIDTH), CONV_KERNEL),
        'w_attn_out': nrm(ks[9], (DEPTH, ATTN_WIDTH, D_MODEL), ATTN_WIDTH),
        'w_conv_out': nrm(ks[10], (DEPTH, CONV_WIDTH, D_MODEL), CONV_WIDTH),
        'w_out': nrm(ks[11], (DEPTH, D_MODEL, D_MODEL), D_MODEL),
        'ffn2_norm': gain(ks[12]),
        'ffn2_w_gate': nrm(ks[13], (DEPTH, D_MODEL, D_FF), D_MODEL),
        'ffn2_w_up': nrm(ks[14], (DEPTH, D_MODEL, D_FF), D_MODEL),
        'ffn2_w_down': nrm(ks[15], (DEPTH, D_FF, D_MODEL), D_FF),
        'final_norm': 1.0 + 0.01 * jax.random.normal(ks[16], (D_MODEL,), jnp.float32),
    }


def reference(x, ffn1_norm, ffn1_w_gate, ffn1_w_up, ffn1_w_down, mix_norm, w_in, b_gate,
              conv_w, w_attn_out, w_conv_out, w_out, ffn2_norm, ffn2_w_gate, ffn2_w_up,
              ffn2_w_down, final_norm):
    for l in range(DEPTH):
        x = x + 0.5 * swiglu(rms_norm(x, ffn1_norm[l]), ffn1_w_gate[l], ffn1_w_up[l], ffn1_w_down[l])
        x = x + hybrid_mixer(rms_norm(x, mix_norm[l]), w_in[l], b_gate[l], conv_w[l],
                             w_attn_out[l], w_conv_out[l], w_out[l])
        x = x + 0.5 * swiglu(rms_norm(x, ffn2_norm[l]), ffn2_w_gate[l], ffn2_w_up[l], ffn2_w_down[l])
    return rms_norm(x, final_norm)
```

```python
import contextlib
import numpy as np
import ml_dtypes
import concourse.bass as bass
import concourse.mybir as mybir
from concourse.bass_utils import run_bass_kernel_spmd

F32 = mybir.dt.float32
BF16 = mybir.dt.bfloat16
AF = mybir.ActivationFunctionType
ALU = mybir.AluOpType
AX = mybir.AxisListType

D = 2048
T = 1024
KC = 16
DFF = 5632
INW = 16384
NSLOT = 3
SLOT_E = 4096
EPS = 1e-6
SCALE = 128 ** -0.5
ENGS = ("pe", "act", "dve", "pool", "sp")
SLOPES = [float(2.0 ** (-8.0 * (h + 1) / 16)) for h in range(16)]
DEBUG_STOP = None
V_N1, V_NM, V_N2, V_NF, V_BG, V_CW, V_ODD, V_W, V_TOT = 0, 16, 32, 48, 64, 96, 144, 145, 161


class Plan:
    def __init__(self):
        self.streams = {e: [] for e in ENGS}
        self.tick = {e: 0 for e in ENGS}
        self.seen = {e: {} for e in ENGS}
        self.state = {}
        self.dval = {}

    def _deps(self, reads, writes):
        deps = {}

        def add(d):
            for s, v in d.items():
                if deps.get(s, 0) < v:
                    deps[s] = v
        for k in reads:
            st = self.state.get(k)
            if st:
                add(st[0])
        for k in writes:
            st = self.state.get(k)
            if st:
                add(st[0])
                add(st[1])
        return deps

    def _filter(self, eng, deps):
        waits = []
        seen = self.seen[eng]
        for s, v in deps.items():
            if s == "pe" and eng == "pe":
                continue
            if seen.get(s, 0) >= v:
                continue
            seen[s] = v
            waits.append((s, v))
        return waits

    def _record(self, tok, reads, writes):
        s, v = tok
        for k in reads:
            st = self.state.setdefault(k, [{}, {}])
            if st[1].get(s, 0) < v:
                st[1][s] = v
        for k in writes:
            self.state[k] = [{s: v}, {}]

    def op(self, eng, emit, reads=(), writes=()):
        waits = self._filter(eng, self._deps(reads, writes))
        self.tick[eng] += 1
        tok = (eng, self.tick[eng])
        self.streams[eng].append((waits, emit, (eng, 1)))
        self._record(tok, reads, writes)
        return tok

    def dma(self, q, emit, sem, reads=(), writes=(), inc=16):
        waits = self._filter(q, self._deps(reads, writes))
        self.dval[sem] = self.dval.get(sem, 0) + inc
        tok = (sem, self.dval[sem])
        self.streams[q].append((waits, emit, (sem, inc)))
        self._record(tok, reads, writes)
        return tok

    def retarget(self, old, new):
        w, r = {}, {}
        for k in old:
            st = self.state.get(k)
            if st:
                for s, v in st[0].items():
                    w[s] = max(w.get(s, 0), v)
                for s, v in st[1].items():
                    r[s] = max(r.get(s, 0), v)
        for k in new:
            self.state[k] = [dict(w), dict(r)]


def build_program(stages=("ffn1", "mixer", "ffn2", "final"), ncores=8):
    nc = bass.Bass("TRN2", target_bir_lowering=False)
    P = Plan()

    def din(name, shape, dt=F32):
        return nc.dram_tensor(name, shape, dt, kind="ExternalInput").ap()

    xT = din("xT", [D, T])
    WSHAPES = {"ffn1_w_gate": [D, DFF], "ffn1_w_up": [D, DFF], "ffn1_w_down": [DFF, D], "w_in": [D, INW],
               "w_attn_out": [D, D], "w_conv_out": [D, D], "w_out": [D, D], "ffn2_w_gate": [D, DFF],
               "ffn2_w_up": [D, DFF], "ffn2_w_down": [DFF, D]}

    class _WD(dict):
        def __missing__(self, nm):
            self[nm] = din(nm, WSHAPES[nm]).rearrange("(kc p) f -> p kc f", p=128)
            return self[nm]
    wd = _WD()
    vecs_d = din("vecs", [128, V_TOT])
    gmask_d = din("gmask", [128, 64])
    tri_d = din("tri", [128, 128], BF16)
    ident_d = din("ident", [128, 128], BF16)
    outT = nc.dram_tensor("outT", [D, T], F32, kind="ExternalOutput").ap()
    bounceU = nc.dram_tensor("bounceU", [128, 32], BF16, kind="Internal").ap()
    gathU = nc.dram_tensor("gathU", [256, 32], BF16, kind="Internal").ap()
    bounceK = [nc.dram_tensor(f"bounceK{g}", [1024, 1024], BF16, kind="Internal").ap() for g in range(2)]
    gathK = [nc.dram_tensor(f"gathK{g}", [2048, 1024], BF16, kind="Internal").ap() for g in range(2)]
    bounceV = [nc.dram_tensor(f"bounceV{g}", [1024, 1024], BF16, kind="Internal").ap() for g in range(2)]
    gathV = [nc.dram_tensor(f"gathV{g}", [2048, 1024], BF16, kind="Internal").ap() for g in range(2)]
    RG = [[2 * i, 2 * i + 1] for i in range(ncores // 2)]

    es = contextlib.ExitStack()
    with es:
        def sb(name, shape, dt):
            return es.enter_context(nc.sbuf_tensor(name, shape, dt))
        X = sb("X", [128, KC, T], F32)
        H = sb("H", [128, KC, T], BF16)
        R = sb("R", [128, 32 * T], BF16)
        W = sb("W", [128, NSLOT * SLOT_E], BF16)
        KT = sb("KT", [128, 2048], BF16)
        VV = sb("VV", [128, 16, 129], BF16)
        SCR = sb("SCR", [128, 2048], F32)
        vecs = sb("vecs_s", [128, V_TOT], F32)
        onec = sb("onec", [128, 16], F32)
        gmask = sb("gmask_s", [128, 64], F32)
        tri = sb("tri_s", [128, 128], BF16)
        ident = sb("ident_s", [128, 128], BF16)
        ones = sb("ones_s", [128, 128], BF16)
        utail = sb("utail", [128, 32], BF16)
        uhb = sb("uhb", [128, 32], BF16)
        uh = sb("uh", [128, 32], F32)
        PS = [es.enter_context(nc.psum_tensor(f"ps{i}", [128, 512], F32)) for i in range(8)]

        sem_names = list(ENGS) + [f"w{i}" for i in range(NSLOT)] + ["ld", "kst0", "kst1", "vst", "ut", "cc0", "ccK0", "ccK1", "ccV0", "ccV1", "v2", "v3",
                                                                  "uh", "kt0", "kt1", "kt2", "kt3", "v0", "v1", "v4", "v5", "v6", "v7", "out"]
        sems = {n: es.enter_context(nc.semaphore(n)) for n in sem_names}

        Rv = R[:, :].rearrange("p (c t) -> p c t", t=T)
        S1c, S2c = 0, 16

        def scr_f32(off, n):
            return SCR[:, off:off + n]

        def scr_bf(off_f32, n_bf):
            return SCR[:, off_f32:off_f32 + (n_bf + 1) // 2].bitcast(BF16)[:, 0:n_bf]

        wviews = []
        wstate = {"issued": 0, "consumed": 0, "released": 0, "hold": False, "hold_idx": 0}

        def slot_view(s, kcn, fw):
            return W[:, s * SLOT_E: s * SLOT_E + kcn * fw].rearrange("p (k f) -> p k f", f=fw)

        def w_issue():
            lim = wstate["hold_idx"] if wstate["hold"] else len(wviews)
            while wstate["issued"] < min(len(wviews), lim) and wstate["issued"] < wstate["released"] + NSLOT:
                i = wstate["issued"]
                s = i % NSLOT
                v = wviews[i]
                dst = slot_view(s, v.shape[1], v.shape[2])
                P.dma("pool", (lambda e, dst=dst, v=v: e.dma_start(out=dst, in_=v)), f"w{s}", writes=[("w", s)])
                wstate["issued"] += 1

        def w_get():
            w_issue()
            n = wstate["consumed"]
            assert n < wstate["issued"]
            s = n % NSLOT
            v = wviews[n]
            wstate["consumed"] += 1
            return s, slot_view(s, v.shape[1], v.shape[2])

        def w_rel():
            wstate["released"] += 1
            w_issue()

        pair_rr = [0]

        def next_pair():
            p = pair_rr[0] % 4
            pair_rr[0] += 1
            return p

        def pkeys(p):
            return [("ps", 2 * p), ("ps", 2 * p + 1)]

        def mm_group(pair, terms, reads, first=True, last=True):
            n = len(terms)

            def emit(e):
                ins = None
                for i, (l, rf) in enumerate(terms):
                    for tt in range(2):
                        ins = e.matmul(PS[2 * pair + tt][:, :], l, rf(tt), start=(first and i == 0),
                                       stop=(last and i == n - 1))
                return ins
            P.op("pe", emit, reads=reads, writes=pkeys(pair))

        def proj_terms(sv, f, kcn, src3, c0=0):
            return [(sv[:, k, f * 128:(f + 1) * 128],
                     (lambda tt, k=k: src3[:, c0 + k, tt * 512:(tt + 1) * 512])) for k in range(kcn)]

        Hk = [("H", c) for c in range(KC)]

        def Rk(c0, n):
            return [("R", c) for c in range(c0, c0 + n)]


        def ACT(out, in_, func, reads, writes, **kw):
            P.op("act", (lambda e: e.activation(out=out, in_=in_, func=func, **kw)), reads=reads, writes=writes)

        def TT(out, in0, in1, op, reads, writes):
            P.op("dve", (lambda e: e.tensor_tensor(out=out, in0=in0, in1=in1, op=op)), reads=reads, writes=writes)

        def TS(out, in0, s1, s2, op0, op1, reads, writes):
            if op1 is None:
                P.op("dve", (lambda e: e.tensor_scalar(out=out, in0=in0, scalar1=s1, scalar2=None, op0=op0)),
                     reads=reads, writes=writes)
            else:
                P.op("dve", (lambda e: e.tensor_scalar(out=out, in0=in0, scalar1=s1, scalar2=s2, op0=op0, op1=op1)),
                     reads=reads, writes=writes)

        def STT(out, in0, scalar, in1, op0, op1, reads, writes):
            P.op("dve", (lambda e: e.scalar_tensor_tensor(out=out, in0=in0, scalar=scalar, in1=in1, op0=op0,
                                                           op1=op1)), reads=reads, writes=writes)

        def CP(out, in_, reads, writes):
            P.op("dve", (lambda e: e.tensor_copy(out=out, in_=in_)), reads=reads, writes=writes)

        def MM1(out, lhsT, rhs, start, stop, reads, writes):
            P.op("pe", (lambda e: e.matmul(out, lhsT, rhs, start=start, stop=stop)), reads=reads, writes=writes)

        def DMA(q, out, in_, sem, reads=(), writes=()):
            P.dma(q, (lambda e: e.dma_start(out=out, in_=in_)), sem, reads=reads, writes=writes)

        scr_keys = {"cur": []}

        def scr_phase(keys):
            P.retarget(scr_keys["cur"], keys)
            scr_keys["cur"] = list(keys)

        xv = xT.rearrange("(c p) t -> p c t", p=128)
        for c in range(KC):
            DMA("sp", X[:, c, :], xv[:, c, :], "ld", writes=[("X", c)])
        DMA("sp", vecs[:, :], vecs_d, "ld", writes=[("vecs",)])
        DMA("sp", gmask[:, :], gmask_d, "ld", writes=[("gmask",)])
        DMA("sp", tri[:, :], tri_d, "ld", writes=[("tri",)])
        DMA("sp", ident[:, :], ident_d, "ld", writes=[("ident",)])
        ld_total = P.dval["ld"]
        for k in [("X", c) for c in range(KC)] + [("vecs",), ("gmask",), ("tri",), ("ident",)]:
            P.state[k] = [{"ld": ld_total}, {}]
        P.op("dve", lambda e: e.memset(ones[:, :], 1.0), writes=[("ones",)])
        P.op("dve", lambda e: e.memset(onec[:, :], 1.0), writes=[("onec",)])
        P.op("dve", lambda e: e.memset(VV[:, :, 128:129], 1.0), writes=[("V", g) for g in range(4)])

        def tsl(tt):
            return slice(tt * 512, (tt + 1) * 512)

        def rmsnorm(gcol, final=False):
            scr_phase([("rs", 0), ("rs", 1), ("sq", 0), ("sq", 1)])
            rs = [scr_f32(0, 512), scr_f32(512, 512)]
            sq = [scr_bf(1024, 512), scr_bf(1280, 512)]
            for tt in range(2):
                for c in range(KC):
                    q = c % 2
                    ACT(sq[q], X[:, c, tsl(tt)], AF.Square, reads=[("X", c)], writes=[("sq", q)])
                    MM1(PS[tt][:, :], ones[:, :], sq[q], c == 0, c == KC - 1, reads=[("sq", q), ("ones",)],
                        writes=[("ps", tt)])
                TS(rs[tt], PS[tt][:, :], 1.0 / D, EPS, ALU.mult, ALU.add, reads=[("ps", tt)], writes=[("rs", tt)])
                ACT(rs[tt], rs[tt], AF.Sqrt, reads=[("rs", tt)], writes=[("rs", tt)])
                P.op("dve", (lambda e, o=rs[tt]: e.reciprocal(out=o, in_=o)), reads=[("rs", tt)], writes=[("rs", tt)])
            for tt in range(2):
                for c in range(KC):
                    g = vecs[:, gcol + c:gcol + c + 1]
                    if final:
                        STT(X[:, c, tsl(tt)], X[:, c, tsl(tt)], g, rs[tt], ALU.mult, ALU.mult,
                            reads=[("X", c), ("rs", tt), ("vecs",)], writes=[("X", c)])
                    else:
                        STT(H[:, c, tsl(tt)], X[:, c, tsl(tt)], g, rs[tt], ALU.mult, ALU.mult,
                            reads=[("X", c), ("rs", tt), ("vecs",)], writes=[("H", c)])

        def ffn_views(wg, wu, wdn):
            vs = []
            for part in range(2):
                for fp in range(11):
                    c0 = (part * 22 + fp * 2) * 128
                    vs.append(wd[wg][:, :, c0:c0 + 256])
                    vs.append(wd[wu][:, :, c0:c0 + 256])
                for dp in range(8):
                    for half in range(2):
                        k0 = part * 22 + half * 11
                        vs.append(wd[wdn][:, k0:k0 + 11, dp * 256:(dp + 1) * 256])
            return vs

        tmp4 = [scr_f32(i * 512, 512) for i in range(4)]
        tmpk = [("tmp", i) for i in range(4)]
        trr = [0]

        def next_tmp():
            ti = trr[0] % 4
            trr[0] += 1
            return ti

        def ffn():
            scr_phase(tmpk)
            for part in range(2):
                for fp in range(11):
                    sg, svg = w_get()
                    su, svu = w_get()
                    pg = [next_pair(), next_pair()]
                    pu = [next_pair(), next_pair()]
                    for f in range(2):
                        mm_group(pg[f], proj_terms(svg, f, KC, H), reads=Hk + [("w", sg)])
                    w_rel()
                    for f in range(2):
                        mm_group(pu[f], proj_terms(svu, f, KC, H), reads=Hk + [("w", su)])
                        fl = fp * 2 + f
                        for tt in range(2):
                            ti = next_tmp()
                            ACT(tmp4[ti], PS[2 * pg[f] + tt][:, :], AF.Silu, reads=[("ps", 2 * pg[f] + tt)],
                                writes=[("tmp", ti)])
                            TT(Rv[:, fl, tsl(tt)], tmp4[ti], PS[2 * pu[f] + tt][:, :], ALU.mult,
                               reads=[("tmp", ti), ("ps", 2 * pu[f] + tt)], writes=[("R", fl)])
                    w_rel()
                for dp in range(8):
                    s0, sv0 = w_get()
                    s1, sv1 = w_get()
                    pd = [next_pair(), next_pair()]
                    for half, (s_, sv_) in enumerate(((s0, sv0), (s1, sv1))):
                        for f in range(2):
                            mm_group(pd[f], proj_terms(sv_, f, 11, Rv, c0=half * 11),
                                     reads=Rk(half * 11, 11) + [("w", s_)], first=(half == 0), last=(half == 1))
                        w_rel()
                    for f in range(2):
                        d = dp * 2 + f
                        for tt in range(2):
                            STT(X[:, d, tsl(tt)], PS[2 * pd[f] + tt][:, :], 0.5, X[:, d, tsl(tt)], ALU.mult, ALU.add,
                                reads=[("ps", 2 * pd[f] + tt), ("X", d)], writes=[("X", d)])

        OFF_Q, OFF_K, OFF_V, OFF_GB, OFF_GC, OFF_XT, OFF_GA, OFF_GCV = 0, 2048, 4096, 6144, 8192, 10240, 12288, 14336

        def wcols(w, off, cp):
            return w[:, :, off + cp * 256: off + (cp + 1) * 256]

        def mixer_views():
            vs = []
            for cp in range(8):
                vs.append(wcols(wd["w_in"], OFF_GC, cp))
                vs.append(wcols(wd["w_in"], OFF_XT, cp))
            for cp in range(8):
                vs.append(wcols(wd["w_in"], OFF_K, cp))
            for cp in range(8):
                vs.append(wcols(wd["w_in"], OFF_V, cp))
            for cp in range(8):
                vs.append(wcols(wd["w_in"], OFF_GB, cp))
            for cp in range(8):
                vs.append(wcols(wd["w_conv_out"], 0, cp))
                vs.append(wcols(wd["w_in"], OFF_GCV, cp))
            for cp in range(8):
                vs.append(wcols(wd["w_in"], OFF_Q, cp))
            for cp in range(8):
                vs.append(wcols(wd["w_attn_out"], 0, cp))
                vs.append(wcols(wd["w_in"], OFF_GA, cp))
            for cp in range(8):
                vs.append(wcols(wd["w_out"], 0, cp))
            return vs

        def evac(i, out, in_, reads, writes):
            if i % 2 == 0:
                ACT(out, in_, AF.Copy, reads=reads, writes=writes)
            else:
                CP(out, in_, reads=reads, writes=writes)

        def mixer():
            scr_phase(tmpk)
            for cp in range(8):
                sa, sva = w_get()
                sb_, svb = w_get()
                pa = [next_pair(), next_pair()]
                pb = [next_pair(), next_pair()]
                for f in range(2):
                    mm_group(pa[f], proj_terms(sva, f, KC, H), reads=Hk + [("w", sa)])
                w_rel()
                for f in range(2):
                    mm_group(pb[f], proj_terms(svb, f, KC, H), reads=Hk + [("w", sb_)])
                    c = cp * 2 + f
                    for tt in range(2):
                        ti = next_tmp()
                        ACT(tmp4[ti], PS[2 * pa[f] + tt][:, :], AF.Copy, reads=[("ps", 2 * pa[f] + tt)],
                            writes=[("tmp", ti)])
                        TT(Rv[:, S1c + c, tsl(tt)], tmp4[ti], PS[2 * pb[f] + tt][:, :], ALU.mult,
                           reads=[("tmp", ti), ("ps", 2 * pb[f] + tt)], writes=[("R", S1c + c)])
                w_rel()
            CP(utail[:, :].rearrange("p (c t) -> p c t", t=2), Rv[:, S1c:S1c + 16, 1022:1024],
               reads=Rk(S1c, 16), writes=[("utail",)])
            DMA("sp", bounceU, utail[:, :], "ut", reads=[("utail",)], writes=[("bounceU",)])
            P.dma("pool", (lambda e: e.collective_compute("AllGather", ALU.bypass, replica_groups=RG,
                                                           ins=[bounceU], outs=[gathU])), "cc0",
                  reads=[("bounceU",)], writes=[("gathU",)], inc=1)
            if DEBUG_STOP == "AG0":
                return
            scr_phase([("kst", 0), ("kst", 1)])
            kst = [scr_bf(0, 1024), scr_bf(512, 1024)]
            kr = 0
            for cp in range(8):
                s, sv = w_get()
                pk = [next_pair(), next_pair()]
                for f in range(2):
                    mm_group(pk[f], proj_terms(sv, f, KC, H), reads=Hk + [("w", s)])
                    h = cp * 2 + f
                    ki = kr % 2
                    kr += 1
                    for tt in range(2):
                        evac(tt, kst[ki][:, tsl(tt)], PS[2 * pk[f] + tt][:, :], reads=[("ps", 2 * pk[f] + tt)],
                             writes=[("kst", ki)])
                    DMA("sp", bounceK[h // 8][(h % 8) * 128:(h % 8 + 1) * 128, :], kst[ki], f"kst{ki}",
                        reads=[("kst", ki)], writes=[("bounceK", h)])
                    if h % 8 == 7:
                        g = h // 8
                        P.dma("pool", (lambda e, g=g: e.collective_compute(
                            "AllGather", ALU.bypass, replica_groups=RG, ins=[bounceK[g]], outs=[gathK[g]])),
                            f"ccK{g}", reads=[("bounceK", hh) for hh in range(g * 8, g * 8 + 8)],
                            writes=[("gathK", g)], inc=1)
                w_rel()
            if DEBUG_STOP == "M2":
                return
            Vst = R[:, S2c * T:(S2c + 16) * T].rearrange("p (tc f) -> p tc f", f=2048)
            hb = 0
            for cp in range(8):
                s, sv = w_get()
                for tcn in range(8):
                    bank = hb % 8
                    hb += 1
                    pv = PS[bank][:, 0:256]

                    def emit(e, tcn=tcn, pv=pv, sv=sv):
                        ins = None
                        for k in range(KC):
                            ins = e.matmul(pv, H[:, k, tcn * 128:(tcn + 1) * 128], sv[:, k, 0:256],
                                           start=(k == 0), stop=(k == KC - 1))
                        return ins
                    P.op("pe", emit, reads=Hk + [("w", s)], writes=[("ps", bank)])
                    evac(tcn, Vst[:, tcn, cp * 256:(cp + 1) * 256], pv, reads=[("ps", bank)],
                         writes=[("R", S2c + 2 * tcn), ("R", S2c + 2 * tcn + 1)])
                w_rel()
            if DEBUG_STOP == "M3":
                return
            vdr = [bounceV[g].rearrange("(tc p two) c -> p tc two c", p=128, two=2) for g in range(2)]
            for tcn in range(8):
                DMA("sp", vdr[tcn // 4][:, tcn % 4, :, :], Vst[:, tcn, :].rearrange("p (two c) -> p two c", two=2),
                    "vst", reads=[("R", S2c + 2 * tcn), ("R", S2c + 2 * tcn + 1)], writes=[("bounceV", tcn)])
            vtot = P.dval["vst"]
            for tcn in range(8):
                P.state[("bounceV", tcn)][0] = {"vst": vtot}
                for k in (("R", S2c + 2 * tcn), ("R", S2c + 2 * tcn + 1)):
                    P.state[k][1]["vst"] = vtot
            if DEBUG_STOP == "M3d":
                return
            for g in range(2):
                P.dma("pool", (lambda e, g=g: e.collective_compute(
                    "AllGather", ALU.bypass, replica_groups=RG, ins=[bounceV[g]], outs=[gathV[g]])),
                    f"ccV{g}", reads=[("bounceV", t) for t in range(g * 4, g * 4 + 4)],
                    writes=[("gathV", g)], inc=1)
            if DEBUG_STOP == "AG1":
                return
            scr_phase([("cv",)])
            cv = scr_f32(0, 1024)
            DMA("sp", uhb[:, :], gathU[0:128, :], "uh", reads=[("gathU",)], writes=[("uhb",)])
            TS(uh[:, :], uhb[:, :], vecs[:, V_ODD:V_ODD + 1], None, ALU.mult, None, reads=[("uhb",), ("vecs",)],
               writes=[("uh",)])

            def cw(i, c):
                return vecs[:, V_CW + i * 16 + c: V_CW + i * 16 + c + 1]
            for cp in range(8):
                s, sv = w_get()
                pg = [next_pair(), next_pair()]
                for f in range(2):
                    mm_group(pg[f], proj_terms(sv, f, KC, H), reads=Hk + [("w", s)])
                w_rel()
                for f in range(2):
                    c = cp * 2 + f
                    uc = Rv[:, S1c + c, :]
                    rk = [("R", S1c + c)]
                    TS(cv, uc, cw(2, c), None, ALU.mult, None, reads=rk + [("vecs",)], writes=[("cv",)])
                    STT(cv[:, 1:1024], uc[:, 0:1023], cw(1, c), cv[:, 1:1024], ALU.mult, ALU.add,
                        reads=rk + [("cv",)], writes=[("cv",)])
                    STT(cv[:, 2:1024], uc[:, 0:1022], cw(0, c), cv[:, 2:1024], ALU.mult, ALU.add,
                        reads=rk + [("cv",)], writes=[("cv",)])
                    STT(cv[:, 0:1], uh[:, 2 * c + 1:2 * c + 2], cw(1, c), cv[:, 0:1], ALU.mult, ALU.add,
                        reads=[("uh",), ("cv",)], writes=[("cv",)])
                    STT(cv[:, 0:1], uh[:, 2 * c:2 * c + 1], cw(0, c), cv[:, 0:1], ALU.mult, ALU.add,
                        reads=[("uh",), ("cv",)], writes=[("cv",)])
                    STT(cv[:, 1:2], uh[:, 2 * c + 1:2 * c + 2], cw(0, c), cv[:, 1:2], ALU.mult, ALU.add,
                        reads=[("uh",), ("cv",)], writes=[("cv",)])
                    for tt in range(2):
                        TT(Rv[:, S1c + c, tsl(tt)], cv[:, tsl(tt)], PS[2 * pg[f] + tt][:, :], ALU.mult,
                           reads=[("cv",), ("ps", 2 * pg[f] + tt)], writes=[("R", S1c + c)])

            if DEBUG_STOP == "M4c":
                return
            def gated(src_c0, bcol, accumulate):
                scr_phase(tmpk)
                for cp in range(8):
                    sa, sva = w_get()
                    sb_, svb = w_get()
                    pa = [next_pair(), next_pair()]
                    pb = [next_pair(), next_pair()]
                    for f in range(2):
                        mm_group(pa[f], proj_terms(sva, f, KC, Rv, c0=src_c0), reads=Rk(src_c0, 16) + [("w", sa)])
                    w_rel()
                    for f in range(2):
                        mm_group(pb[f], proj_terms(svb, f, KC, H), reads=Hk + [("w", sb_)])
                        c = cp * 2 + f
                        for tt in range(2):
                            ti = next_tmp()
                            ACT(tmp4[ti], PS[2 * pb[f] + tt][:, :], AF.Sigmoid,
                                reads=[("ps", 2 * pb[f] + tt), ("vecs",)], writes=[("tmp", ti)],
                                bias=vecs[:, bcol + c:bcol + c + 1])
                            dst = Rv[:, S2c + c, tsl(tt)]
                            if not accumulate:
                                TT(dst, tmp4[ti], PS[2 * pa[f] + tt][:, :], ALU.mult,
                                   reads=[("tmp", ti), ("ps", 2 * pa[f] + tt)], writes=[("R", S2c + c)])
                            else:
                                TT(tmp4[ti], tmp4[ti], PS[2 * pa[f] + tt][:, :], ALU.mult,
                                   reads=[("tmp", ti), ("ps", 2 * pa[f] + tt)], writes=[("tmp", ti)])
                                TT(dst, tmp4[ti], dst, ALU.add, reads=[("tmp", ti), ("R", S2c + c)],
                                   writes=[("R", S2c + c)])
                    w_rel()
            gated(S1c, V_BG + 16, accumulate=False)
            if DEBUG_STOP == "M5":
                return
            for cp in range(8):
                s, sv = w_get()
                pq = [next_pair(), next_pair()]
                for f in range(2):
                    mm_group(pq[f], proj_terms(sv, f, KC, H), reads=Hk + [("w", s)])
                    h = cp * 2 + f
                    for tt in range(2):
                        evac(tt, Rv[:, S1c + h, tsl(tt)], PS[2 * pq[f] + tt][:, :], reads=[("ps", 2 * pq[f] + tt)],
                             writes=[("R", S1c + h)])
                w_rel()
            if DEBUG_STOP == "M4a":
                return
            attention()
            if DEBUG_STOP == "attn":
                return
            gated(S1c, V_BG, accumulate=True)
            for dp in range(8):
                s, sv = w_get()
                po = [next_pair(), next_pair()]
                for f in range(2):
                    mm_group(po[f], proj_terms(sv, f, KC, Rv, c0=S2c), reads=Rk(S2c, 16) + [("w", s)])
                    d = dp * 2 + f
                    for tt in range(2):
                        TT(X[:, d, tsl(tt)], PS[2 * po[f] + tt][:, :], X[:, d, tsl(tt)], ALU.add,
                           reads=[("ps", 2 * po[f] + tt), ("X", d)], writes=[("X", d)])
                w_rel()

        def attention():
            pT = [scr_bf(i * 128, 256) for i in range(6)]
            acc = [scr_f32(768 + i * 132, 132) for i in range(2)]
            obf = [scr_bf(1032 + i * 64, 128) for i in range(2)]
            rec = [scr_f32(1160 + i, 1) for i in range(2)]
            HB = [1168, 1376]
            gsc = [scr_f32(b_, 64) for b_ in HB]
            sel = [scr_f32(b_ + 64, 64) for b_ in HB]
            thr8 = [scr_f32(b_ + 128, 64) for b_ in HB]
            ksum = [scr_f32(b_ + 192, 8) for b_ in HB]
            kmb = [scr_bf(b_ + 200, 8) for b_ in HB]
            keys = ([("pT", i) for i in range(6)] + [("acc", i) for i in range(2)] + [("obf", i) for i in range(2)] +
                    [("rec", 0), ("rec", 1)] +
                    [(n_, b_) for n_ in ("gsc", "sel", "thr8", "ksum", "kmb") for b_ in range(2)])
            scr_phase(keys)
            for h in range(16):
                P.retarget([("R", S1c + h)], [("Q", h, i) for i in range(8)])
            KTb = [KT[:, :], W[:, 0:2048]]
            VVb = [VV[:, :, :], W[:, SLOT_E:SLOT_E + 16 * 129].rearrange("p (c d) -> p c d", d=129)]
            P.retarget([("KT", 0)], [("KT", 0, 0)])
            P.retarget([("KT", 1)], [("KT", 0, 1)])
            for g in range(4):
                P.retarget([("V", g)], [("V", 0, g)])
            P.retarget([("w", 0)], [("KT", 1, 0), ("KT", 1, 1)])
            P.retarget([("w", 1)], [("V", 1, g) for g in range(4)])
            psS = [PS[b][:, 0:256] for b in (0, 1, 2, 3, 4)]
            psO = [PS[b][:, 0:129] for b in (5, 6)]
            psG = PS[7][:, 0:64]
            psT = PS[7][:, 128:192].bitcast(BF16)
            psSk = [("ps", b) for b in (0, 1, 2, 3, 4)]
            psOk = [("ps", b) for b in (5, 6)]
            vview_g = [gathV[g][0:1024, :].rearrange("(tc p two) c -> p tc (two c)", p=128, two=2) for g in range(2)]
            vview_b = [bounceV[g].rearrange("(tc p two) c -> p tc (two c)", p=128, two=2) for g in range(2)]
            cnt = {"s": 0, "p": 0, "o": 0}
            mod = {"s": 5, "p": 6, "o": 2}

            def rr(k):
                v = cnt[k] % mod[k]
                cnt[k] += 1
                return v

            def load(h, b):
                hs = slice(h * 128, (h + 1) * 128)
                hg, hl = h // 8, slice((h % 8) * 128, (h % 8 + 1) * 128)
                DMA("sp", KTb[b][:, 0:1024], gathK[hg][hl, :], f"kt{2 * b}", reads=[("gathK", hg)],
                    writes=[("KT", b, 0)])
                DMA("sp", KTb[b][:, 1024:2048], bounceK[hg][hl, :], f"kt{2 * b + 1}", reads=[("bounceK", h)],
                    writes=[("KT", b, 1)])
                for g in range(2):
                    DMA("sp", VVb[b][:, 4 * g:4 * g + 4, 0:128], vview_g[g][:, :, hs], f"v{4 * b + g}",
                        reads=[("gathV", g)], writes=[("V", b, g)])
                    DMA("sp", VVb[b][:, 8 + 4 * g:12 + 4 * g, 0:128], vview_b[g][:, :, hs], f"v{4 * b + 2 + g}",
                        reads=[("bounceV", t) for t in range(4 * g, 4 * g + 4)], writes=[("V", b, 2 + g)])

            def setup(h, b):
                wh = vecs[:, V_W + h:V_W + h + 1]
                vk4 = [("V", b, g) for g in range(4)]
                TS(VVb[b][:, :, 0:128], VVb[b][:, :, 0:128], wh, None, ALU.mult, None, reads=vk4 + [("vecs",)],
                   writes=vk4)
                TS(VVb[b][:, :, 128:129], onec[:, :].rearrange("p (c o) -> p c o", o=1), wh, None, ALU.mult, None,
                   reads=vk4 + [("vecs",), ("onec",)], writes=vk4)
                QT = Rv[:, S1c + h, :]
                ktb = KTb[b]
                P.op("dve", (lambda e: e.tensor_reduce(out=ksum[b], in_=ktb.rearrange("p (b k) -> p b k", k=256),
                                                        axis=AX.X, op=ALU.add)),
                     reads=[("KT", b, 0), ("KT", b, 1)], writes=[("ksum", b)])
                TS(kmb[b], ksum[b], 1.0 / 256, None, ALU.mult, None, reads=[("ksum", b)], writes=[("kmb", b)])

                def emit_gate(e):
                    ins = None
                    for i in range(8):
                        ins = e.matmul(psG[:, i * 8:(i + 1) * 8], QT[:, i * 128:(i + 1) * 128], kmb[b], start=True,
                                       stop=True)
                    return ins
                P.op("pe", emit_gate, reads=[("Q", h, i) for i in range(8)] + [("kmb", b)], writes=[("ps", 7)])
                TT(gsc[b], psG, gmask[:, :], ALU.add, reads=[("ps", 7), ("gmask",)], writes=[("gsc", b)])
                for i in range(8):
                    P.op("dve", (lambda e, i=i: e.max(out=thr8[b][:, i * 8:(i + 1) * 8],
                                                      in_=gsc[b][:, i * 8:(i + 1) * 8])),
                         reads=[("gsc", b)], writes=[("thr8", b)])
                for i in range(8):
                    TS(sel[b][:, i * 8:(i + 1) * 8], gsc[b][:, i * 8:(i + 1) * 8], thr8[b][:, i * 8 + 2:i * 8 + 3],
                       None, ALU.is_ge, None, reads=[("gsc", b), ("thr8", b)], writes=[("sel", b)])
                STT(sel[b], gsc[b], -1.0e29, sel[b], ALU.is_gt, ALU.mult, reads=[("gsc", b), ("sel", b)],
                    writes=[("sel", b)])

            load(0, 0)
            setup(0, 0)
            for h in range(16):
                hb = h % 2
                if h + 1 < 16:
                    load(h + 1, 1 - hb)
                QT = Rv[:, S1c + h, :]
                blist = []
                for m_ in range(4):
                    bl = [0, 1, 2, 3] + list(range(4, 4 + m_ + 1))
                    for bi, j in enumerate(bl):
                        for i in (2 * m_, 2 * m_ + 1):
                            diag = (j == 4 + i // 2)
                            kcs = [0] if (diag and i % 2 == 0) else [0, 1]
                            blist.append(dict(i=i, bi=bi, j=j, diag=diag, kcs=kcs, last=(bi == len(bl) - 1)))

                def front(b, h=h, QT=QT, hb=hb):
                    i, j, kcs = b["i"], b["j"], b["kcs"]
                    qs = slice(i * 128, (i + 1) * 128)
                    si, pi = rr("s"), rr("p")
                    b["pi"] = pi
                    kkey = ("KT", hb, 0) if j < 4 else ("KT", hb, 1)
                    ktb = KTb[hb]

                    def emit_s(e):
                        ins = None
                        for kc in kcs:
                            vk = 2 * j + kc
                            ins = e.matmul(psS[si][:, kc * 128:(kc + 1) * 128], ktb[:, vk * 128:(vk + 1) * 128],
                                           QT[:, qs], start=True, stop=True)
                        return ins
                    P.op("pe", emit_s, reads=[kkey, ("Q", h, i)], writes=[psSk[si]])
                    for kc in kcs:
                        dc = 8 + i - (2 * j + kc)
                        ACT(pT[pi][:, kc * 128:(kc + 1) * 128], psS[si][:, kc * 128:(kc + 1) * 128], AF.Exp,
                            reads=[psSk[si]], writes=[("pT", pi)], scale=SCALE,
                            bias=float(-SLOPES[h] * 128.0 * dc))
                    if b["diag"]:
                        kd = i % 2
                        TT(pT[pi][:, kd * 128:(kd + 1) * 128], pT[pi][:, kd * 128:(kd + 1) * 128], tri[:, :],
                           ALU.mult, reads=[("pT", pi), ("tri",)], writes=[("pT", pi)])

                def back(b, h=h, QT=QT, hb=hb):
                    i, j, kcs, pi = b["i"], b["j"], b["kcs"], b["pi"]
                    qs = slice(i * 128, (i + 1) * 128)
                    ai = i % 2
                    oi = rr("o")
                    vkey = ("V", hb, j // 2)
                    vvb = VVb[hb]

                    def emit_pv(e):
                        ins = None
                        for n, kc in enumerate(kcs):
                            vk = 2 * j + kc
                            ins = e.matmul(psO[oi], pT[pi][:, kc * 128:(kc + 1) * 128], vvb[:, vk, 0:129],
                                           start=(n == 0), stop=(n == len(kcs) - 1))
                        return ins
                    P.op("pe", emit_pv, reads=[("pT", pi), vkey], writes=[psOk[oi]])
                    sc = sel[hb][:, i * 8 + j:i * 8 + j + 1]
                    a129 = acc[ai][:, 0:129]
                    if b["bi"] == 0:
                        TS(a129, psO[oi], sc, None, ALU.mult, None, reads=[psOk[oi], ("sel", hb)],
                           writes=[("acc", ai)])
                    elif b["diag"]:
                        TT(a129, psO[oi], a129, ALU.add, reads=[psOk[oi], ("acc", ai)], writes=[("acc", ai)])
                    else:
                        STT(a129, psO[oi], sc, a129, ALU.mult, ALU.add,
                            reads=[psOk[oi], ("sel", hb), ("acc", ai)], writes=[("acc", ai)])
                    if b["last"]:
                        P.op("dve", (lambda e: e.reciprocal(out=rec[ai], in_=acc[ai][:, 128:129])),
                             reads=[("acc", ai)], writes=[("rec", ai)])
                        TS(obf[ai], acc[ai][:, 0:128], rec[ai], None, ALU.mult, None,
                           reads=[("acc", ai), ("rec", ai)], writes=[("obf", ai)])

                        def fin():
                            P.op("pe", (lambda e: e.transpose(psT, obf[ai], ident[:, :])),
                                 reads=[("obf", ai), ("ident",)], writes=[("ps", 7)])
                            ACT(QT[:, qs], psT, AF.Copy, reads=[("ps", 7)], writes=[("Q", h, i)])
                        return fin
                    return None
                LA = 4
                pend = []
                nb_ = len(blist)
                for idx in range(nb_ + LA):
                    if idx == 16 and h + 1 < 16:
                        setup(h + 1, 1 - hb)
                    if idx < nb_:
                        front(blist[idx])
                    if idx >= LA:
                        fin = back(blist[idx - LA])
                        while pend and pend[0][0] <= idx:
                            pend.pop(0)[1]()
                        if fin is not None:
                            pend.append((idx + 2, fin))
                for _, f_ in pend:
                    f_()
            for h in range(16):
                P.retarget([("Q", h, i) for i in range(8)], [("R", S1c + h)])
            P.retarget([("KT", 1, 0), ("KT", 1, 1)], [("w", 0)])
            P.retarget([("V", 1, g) for g in range(4)], [("w", 1)])
            wstate["hold"] = False

        if "ffn1" in stages:
            wviews.extend(ffn_views("ffn1_w_gate", "ffn1_w_up", "ffn1_w_down"))
        if "mixer" in stages:
            wstate["hold_idx"] = len(wviews) + 64
            wstate["hold"] = True
            wviews.extend(mixer_views())
        if "ffn2" in stages:
            wviews.extend(ffn_views("ffn2_w_gate", "ffn2_w_up", "ffn2_w_down"))

        if "ffn1" in stages:
            rmsnorm(V_N1)
            ffn()
        if "mixer" in stages:
            rmsnorm(V_NM)
            mixer()
        if "ffn2" in stages:
            rmsnorm(V_N2)
            ffn()
        if "final" in stages:
            rmsnorm(V_NF, final=True)
        ov = outT.rearrange("(c p) t -> p c t", p=128)
        for c in range(KC):
            P.dma("sp", (lambda e, c=c: e.dma_start(out=ov[:, c, :], in_=X[:, c, :])), "out", reads=[("X", c)])
        out_total = P.dval["out"]
        P.streams["sp"].append(([("out", out_total)], None, None))
        assert DEBUG_STOP or wstate["consumed"] == len(wviews), (wstate, len(wviews))

        def run_stream(name, e):
            for waits, emit, sig in P.streams[name]:
                for s, v in waits:
                    e.wait_ge(sems[s], v)
                if emit is None:
                    continue
                ins = emit(e)
                ins.then_inc(sems[sig[0]], sig[1])

        with nc.Block() as block:
            @block.tensor
            def _(e):
                run_stream("pe", e)

            @block.scalar
            def _(e):
                run_stream("act", e)

            @block.vector
            def _(e):
                run_stream("dve", e)

            @block.gpsimd
            def _(e):
                run_stream("pool", e)

            @block.sync
            def _(e):
                run_stream("sp", e)
    return nc


def _host_consts():
    slopes = np.exp2(-8.0 * np.arange(1, 17, dtype=np.float64) / 16)
    p = np.arange(128, dtype=np.float64)[:, None]
    wtab = np.exp(slopes[None, :] * (p - 64.0)).astype(np.float32)
    k = np.arange(128)[:, None]
    q = np.arange(128)[None, :]
    tri = (k <= q).astype(np.float32).astype(ml_dtypes.bfloat16)
    ident = np.eye(128, dtype=np.float32).astype(ml_dtypes.bfloat16)
    return wtab, tri, ident


def _gmask(odd):
    g = np.full((128, 8, 8), -1.0e30, dtype=np.float32)
    for i in range(8):
        if odd:
            g[:, i, 0:4] = 0.0
        g[:, i, 4:4 + i // 2] = 0.0
    return np.ascontiguousarray(g.reshape(128, 64))


def _pc(v):
    v = np.asarray(v, np.float32)
    return v.reshape(-1, 128).T


def kernel(**inputs):
    x = np.asarray(inputs["x"], np.float32)
    wtab, tri, ident = _host_consts()
    common = {}
    for nm in ("ffn1_w_gate", "ffn1_w_up", "ffn1_w_down", "w_in", "w_attn_out", "w_conv_out", "w_out",
               "ffn2_w_gate", "ffn2_w_up", "ffn2_w_down"):
        common[nm] = np.ascontiguousarray(np.asarray(inputs[nm], np.float32)[0])
    vec = np.zeros((128, V_TOT), np.float32)
    vec[:, V_N1:V_N1 + 16] = _pc(inputs["ffn1_norm"][0])
    vec[:, V_NM:V_NM + 16] = _pc(inputs["mix_norm"][0])
    vec[:, V_N2:V_N2 + 16] = _pc(inputs["ffn2_norm"][0])
    vec[:, V_NF:V_NF + 16] = _pc(inputs["final_norm"])
    vec[:, V_BG:V_BG + 32] = _pc(inputs["b_gate"][0])
    cwv = np.asarray(inputs["conv_w"], np.float32)[0]
    for i in range(3):
        vec[:, V_CW + 16 * i:V_CW + 16 * (i + 1)] = _pc(cwv[i])
    vec[:, V_W:V_W + 16] = wtab
    common.update(tri=tri, ident=ident)
    in_maps = []
    for c in range(8):
        b, half = c // 2, c % 2
        m = dict(common)
        m["xT"] = np.ascontiguousarray(x[b, half * T:(half + 1) * T, :].T)
        v = vec.copy()
        v[:, V_ODD] = float(half)
        m["vecs"] = v
        m["gmask"] = _gmask(half == 1)
        in_maps.append(m)
    nc = build_program()
    res = run_bass_kernel_spmd(nc, in_maps, core_ids=list(range(8)))
    out = np.empty((4, 2048, D), np.float32)
    for c in range(8):
        b, half = c // 2, c % 2
        out[b, half * T:(half + 1) * T, :] = np.asarray(res.results[c]["outT"], np.float32).T
    return out
```

```python
import contextlib
import numpy as np
import ml_dtypes
import concourse.bass as bass
import concourse.mybir as mybir
from concourse.bass_utils import run_bass_kernel_spmd

F32 = mybir.dt.float32
BF16 = mybir.dt.bfloat16
AF = mybir.ActivationFunctionType
ALU = mybir.AluOpType
AX = mybir.AxisListType

D = 2048
T = 1024
KC = 16
DFF = 5632
INW = 16384
NSLOT = 3
SLOT_E = 4096
EPS = 1e-6
SCALE = 128 ** -0.5
ENGS = ("pe", "act", "dve", "pool", "sp")
SLOPES = [float(2.0 ** (-8.0 * (h + 1) / 16)) for h in range(16)]
DEBUG_STOP = None
V_N1, V_NM, V_N2, V_NF, V_BG, V_CW, V_ODD, V_W, V_TOT = 0, 16, 32, 48, 64, 96, 144, 145, 161


class Plan:
    def __init__(self):
        self.streams = {e: [] for e in ENGS}
        self.tick = {e: 0 for e in ENGS}
        self.seen = {e: {} for e in ENGS}
        self.state = {}
        self.dval = {}

    def _deps(self, reads, writes):
        deps = {}

        def add(d):
            for s, v in d.items():
                if deps.get(s, 0) < v:
                    deps[s] = v
        for k in reads:
            st = self.state.get(k)
            if st:
                add(st[0])
        for k in writes:
            st = self.state.get(k)
            if st:
                add(st[0])
                add(st[1])
        return deps

    def _filter(self, eng, deps):
        waits = []
        seen = self.seen[eng]
        for s, v in deps.items():
            if s == "pe" and eng == "pe":
                continue
            if seen.get(s, 0) >= v:
                continue
            seen[s] = v
            waits.append((s, v))
        return waits

    def _record(self, tok, reads, writes):
        s, v = tok
        for k in reads:
            st = self.state.setdefault(k, [{}, {}])
            if st[1].get(s, 0) < v:
                st[1][s] = v
        for k in writes:
            self.state[k] = [{s: v}, {}]

    def op(self, eng, emit, reads=(), writes=()):
        waits = self._filter(eng, self._deps(reads, writes))
        self.tick[eng] += 1
        tok = (eng, self.tick[eng])
        self.streams[eng].append((waits, emit, (eng, 1)))
        self._record(tok, reads, writes)
        return tok

    def dma(self, q, emit, sem, reads=(), writes=(), inc=16):
        waits = self._filter(q, self._deps(reads, writes))
        self.dval[sem] = self.dval.get(sem, 0) + inc
        tok = (sem, self.dval[sem])
        self.streams[q].append((waits, emit, (sem, inc)))
        self._record(tok, reads, writes)
        return tok

    def retarget(self, old, new):
        w, r = {}, {}
        for k in old:
            st = self.state.get(k)
            if st:
                for s, v in st[0].items():
                    w[s] = max(w.get(s, 0), v)
                for s, v in st[1].items():
                    r[s] = max(r.get(s, 0), v)
        for k in new:
            self.state[k] = [dict(w), dict(r)]


def build_program(stages=("ffn1", "mixer", "ffn2", "final"), ncores=8):
    nc = bass.Bass("TRN2", target_bir_lowering=False)
    P = Plan()

    def din(name, shape, dt=F32):
        return nc.dram_tensor(name, shape, dt, kind="ExternalInput").ap()

    xT = din("xT", [D, T])
    WSHAPES = {"ffn1_w_gate": [D, DFF], "ffn1_w_up": [D, DFF], "ffn1_w_down": [DFF, D], "w_in": [D, INW],
               "w_attn_out": [D, D], "w_conv_out": [D, D], "w_out": [D, D], "ffn2_w_gate": [D, DFF],
               "ffn2_w_up": [D, DFF], "ffn2_w_down": [DFF, D]}

    class _WD(dict):
        def __missing__(self, nm):
            self[nm] = din(nm, WSHAPES[nm]).rearrange("(kc p) f -> p kc f", p=128)
            return self[nm]
    wd = _WD()
    vecs_d = din("vecs", [128, V_TOT])
    gmask_d = din("gmask", [128, 64])
    tri_d = din("tri", [128, 128], BF16)
    ident_d = din("ident", [128, 128], BF16)
    outT = nc.dram_tensor("outT", [D, T], F32, kind="ExternalOutput").ap()
    bounceU = nc.dram_tensor("bounceU", [128, 32], BF16, kind="Internal").ap()
    gathU = nc.dram_tensor("gathU", [256, 32], BF16, kind="Internal").ap()
    bounceK = [nc.dram_tensor(f"bounceK{g}", [1024, 1024], BF16, kind="Internal").ap() for g in range(2)]
    gathK = [nc.dram_tensor(f"gathK{g}", [2048, 1024], BF16, kind="Internal").ap() for g in range(2)]
    bounceV = [nc.dram_tensor(f"bounceV{g}", [1024, 1024], BF16, kind="Internal").ap() for g in range(2)]
    gathV = [nc.dram_tensor(f"gathV{g}", [2048, 1024], BF16, kind="Internal").ap() for g in range(2)]
    RG = [[2 * i, 2 * i + 1] for i in range(ncores // 2)]

    es = contextlib.ExitStack()
    with es:
        def sb(name, shape, dt):
            return es.enter_context(nc.sbuf_tensor(name, shape, dt))
        X = sb("X", [128, KC, T], F32)
        H = sb("H", [128, KC, T], BF16)
        R = sb("R", [128, 32 * T], BF16)
        W = sb("W", [128, NSLOT * SLOT_E], BF16)
        KT = sb("KT", [128, 2048], BF16)
        VV = sb("VV", [128, 16, 129], BF16)
        SCR = sb("SCR", [128, 2048], F32)
        vecs = sb("vecs_s", [128, V_TOT], F32)
        onec = sb("onec", [128, 16], F32)
        gmask = sb("gmask_s", [128, 64], F32)
        tri = sb("tri_s", [128, 128], BF16)
        ident = sb("ident_s", [128, 128], BF16)
        ones = sb("ones_s", [128, 128], BF16)
        utail = sb("utail", [128, 32], BF16)
        uhb = sb("uhb", [128, 32], BF16)
        uh = sb("uh", [128, 32], F32)
        PS = [es.enter_context(nc.psum_tensor(f"ps{i}", [128, 512], F32)) for i in range(8)]

        sem_names = list(ENGS) + [f"w{i}" for i in range(NSLOT)] + ["ld", "kst0", "kst1", "vst", "ut", "cc0", "ccK0", "ccK1", "ccV0", "ccV1", "v2", "v3",
                                                                  "uh", "kt0", "kt1", "kt2", "kt3", "v0", "v1", "v4", "v5", "v6", "v7", "out"]
        sems = {n: es.enter_context(nc.semaphore(n)) for n in sem_names}

        Rv = R[:, :].rearrange("p (c t) -> p c t", t=T)
        S1c, S2c = 0, 16

        def scr_f32(off, n):
            return SCR[:, off:off + n]

        def scr_bf(off_f32, n_bf):
            return SCR[:, off_f32:off_f32 + (n_bf + 1) // 2].bitcast(BF16)[:, 0:n_bf]

        wviews = []
        wstate = {"issued": 0, "consumed": 0, "released": 0, "hold": False, "hold_idx": 0}

        def slot_view(s, kcn, fw):
            return W[:, s * SLOT_E: s * SLOT_E + kcn * fw].rearrange("p (k f) -> p k f", f=fw)

        def w_issue():
            lim = wstate["hold_idx"] if wstate["hold"] else len(wviews)
            while wstate["issued"] < min(len(wviews), lim) and wstate["issued"] < wstate["released"] + NSLOT:
                i = wstate["issued"]
                s = i % NSLOT
                v = wviews[i]
                dst = slot_view(s, v.shape[1], v.shape[2])
                P.dma("pool", (lambda e, dst=dst, v=v: e.dma_start(out=dst, in_=v)), f"w{s}", writes=[("w", s)])
                wstate["issued"] += 1

        def w_get():
            w_issue()
            n = wstate["consumed"]
            assert n < wstate["issued"]
            s = n % NSLOT
            v = wviews[n]
            wstate["consumed"] += 1
            return s, slot_view(s, v.shape[1], v.shape[2])

        def w_rel():
            wstate["released"] += 1
            w_issue()

        pair_rr = [0]

        def next_pair():
            p = pair_rr[0] % 4
            pair_rr[0] += 1
            return p

        def pkeys(p):
            return [("ps", 2 * p), ("ps", 2 * p + 1)]

        def mm_group(pair, terms, reads, first=True, last=True):
            n = len(terms)

            def emit(e):
                ins = None
                for i, (l, rf) in enumerate(terms):
                    for tt in range(2):
                        ins = e.matmul(PS[2 * pair + tt][:, :], l, rf(tt), start=(first and i == 0),
                                       stop=(last and i == n - 1))
                return ins
            P.op("pe", emit, reads=reads, writes=pkeys(pair))

        def proj_terms(sv, f, kcn, src3, c0=0):
            return [(sv[:, k, f * 128:(f + 1) * 128],
                     (lambda tt, k=k: src3[:, c0 + k, tt * 512:(tt + 1) * 512])) for k in range(kcn)]

        Hk = [("H", c) for c in range(KC)]

        def Rk(c0, n):
            return [("R", c) for c in range(c0, c0 + n)]


        def ACT(out, in_, func, reads, writes, **kw):
            P.op("act", (lambda e: e.activation(out=out, in_=in_, func=func, **kw)), reads=reads, writes=writes)

        def TT(out, in0, in1, op, reads, writes):
            P.op("dve", (lambda e: e.tensor_tensor(out=out, in0=in0, in1=in1, op=op)), reads=reads, writes=writes)

        def TS(out, in0, s1, s2, op0, op1, reads, writes):
            if op1 is None:
                P.op("dve", (lambda e: e.tensor_scalar(out=out, in0=in0, scalar1=s1, scalar2=None, op0=op0)),
                     reads=reads, writes=writes)
            else:
                P.op("dve", (lambda e: e.tensor_scalar(out=out, in0=in0, scalar1=s1, scalar2=s2, op0=op0, op1=op1)),
                     reads=reads, writes=writes)

        def STT(out, in0, scalar, in1, op0, op1, reads, writes):
            P.op("dve", (lambda e: e.scalar_tensor_tensor(out=out, in0=in0, scalar=scalar, in1=in1, op0=op0,
                                                           op1=op1)), reads=reads, writes=writes)

        def CP(out, in_, reads, writes):
            P.op("dve", (lambda e: e.tensor_copy(out=out, in_=in_)), reads=reads, writes=writes)

        def MM1(out, lhsT, rhs, start, stop, reads, writes):
            P.op("pe", (lambda e: e.matmul(out, lhsT, rhs, start=start, stop=stop)), reads=reads, writes=writes)

        def DMA(q, out, in_, sem, reads=(), writes=()):
            P.dma(q, (lambda e: e.dma_start(out=out, in_=in_)), sem, reads=reads, writes=writes)

        scr_keys = {"cur": []}

        def scr_phase(keys):
            P.retarget(scr_keys["cur"], keys)
            scr_keys["cur"] = list(keys)

        xv = xT.rearrange("(c p) t -> p c t", p=128)
        for c in range(KC):
            DMA("sp", X[:, c, :], xv[:, c, :], "ld", writes=[("X", c)])
        DMA("sp", vecs[:, :], vecs_d, "ld", writes=[("vecs",)])
        DMA("sp", gmask[:, :], gmask_d, "ld", writes=[("gmask",)])
        DMA("sp", tri[:, :], tri_d, "ld", writes=[("tri",)])
        DMA("sp", ident[:, :], ident_d, "ld", writes=[("ident",)])
        ld_total = P.dval["ld"]
        for k in [("X", c) for c in range(KC)] + [("vecs",), ("gmask",), ("tri",), ("ident",)]:
            P.state[k] = [{"ld": ld_total}, {}]
        P.op("dve", lambda e: e.memset(ones[:, :], 1.0), writes=[("ones",)])
        P.op("dve", lambda e: e.memset(onec[:, :], 1.0), writes=[("onec",)])
        P.op("dve", lambda e: e.memset(VV[:, :, 128:129], 1.0), writes=[("V", g) for g in range(4)])

        def tsl(tt):
            return slice(tt * 512, (tt + 1) * 512)

        def rmsnorm(gcol, final=False):
            scr_phase([("rs", 0), ("rs", 1), ("sq", 0), ("sq", 1)])
            rs = [scr_f32(0, 512), scr_f32(512, 512)]
            sq = [scr_bf(1024, 512), scr_bf(1280, 512)]
            for tt in range(2):
                for c in range(KC):
                    q = c % 2
                    ACT(sq[q], X[:, c, tsl(tt)], AF.Square, reads=[("X", c)], writes=[("sq", q)])
                    MM1(PS[tt][:, :], ones[:, :], sq[q], c == 0, c == KC - 1, reads=[("sq", q), ("ones",)],
                        writes=[("ps", tt)])
                TS(rs[tt], PS[tt][:, :], 1.0 / D, EPS, ALU.mult, ALU.add, reads=[("ps", tt)], writes=[("rs", tt)])
                ACT(rs[tt], rs[tt], AF.Sqrt, reads=[("rs", tt)], writes=[("rs", tt)])
                P.op("dve", (lambda e, o=rs[tt]: e.reciprocal(out=o, in_=o)), reads=[("rs", tt)], writes=[("rs", tt)])
            for tt in range(2):
                for c in range(KC):
                    g = vecs[:, gcol + c:gcol + c + 1]
                    if final:
                        STT(X[:, c, tsl(tt)], X[:, c, tsl(tt)], g, rs[tt], ALU.mult, ALU.mult,
                            reads=[("X", c), ("rs", tt), ("vecs",)], writes=[("X", c)])
                    else:
                        STT(H[:, c, tsl(tt)], X[:, c, tsl(tt)], g, rs[tt], ALU.mult, ALU.mult,
                            reads=[("X", c), ("rs", tt), ("vecs",)], writes=[("H", c)])

        def ffn_views(wg, wu, wdn):
            vs = []
            for part in range(2):
                for fp in range(11):
                    c0 = (part * 22 + fp * 2) * 128
                    vs.append(wd[wg][:, :, c0:c0 + 256])
                    vs.append(wd[wu][:, :, c0:c0 + 256])
                for dp in range(8):
                    for half in range(2):
                        k0 = part * 22 + half * 11
                        vs.append(wd[wdn][:, k0:k0 + 11, dp * 256:(dp + 1) * 256])
            return vs

        tmp4 = [scr_f32(i * 512, 512) for i in range(4)]
        tmpk = [("tmp", i) for i in range(4)]
        trr = [0]

        def next_tmp():
            ti = trr[0] % 4
            trr[0] += 1
            return ti

        def ffn():
            scr_phase(tmpk)
            for part in range(2):
                for fp in range(11):
                    sg, svg = w_get()
                    su, svu = w_get()
                    pg = [next_pair(), next_pair()]
                    pu = [next_pair(), next_pair()]
                    for f in range(2):
                        mm_group(pg[f], proj_terms(svg, f, KC, H), reads=Hk + [("w", sg)])
                    w_rel()
                    for f in range(2):
                        mm_group(pu[f], proj_terms(svu, f, KC, H), reads=Hk + [("w", su)])
                        fl = fp * 2 + f
                        for tt in range(2):
                            ti = next_tmp()
                            ACT(tmp4[ti], PS[2 * pg[f] + tt][:, :], AF.Silu, reads=[("ps", 2 * pg[f] + tt)],
                                writes=[("tmp", ti)])
                            TT(Rv[:, fl, tsl(tt)], tmp4[ti], PS[2 * pu[f] + tt][:, :], ALU.mult,
                               reads=[("tmp", ti), ("ps", 2 * pu[f] + tt)], writes=[("R", fl)])
                    w_rel()
                for dp in range(8):
                    s0, sv0 = w_get()
                    s1, sv1 = w_get()
                    pd = [next_pair(), next_pair()]
                    for half, (s_, sv_) in enumerate(((s0, sv0), (s1, sv1))):
                        for f in range(2):
                            mm_group(pd[f], proj_terms(sv_, f, 11, Rv, c0=half * 11),
                                     reads=Rk(half * 11, 11) + [("w", s_)], first=(half == 0), last=(half == 1))
                        w_rel()
                    for f in range(2):
                        d = dp * 2 + f
                        for tt in range(2):
                            STT(X[:, d, tsl(tt)], PS[2 * pd[f] + tt][:, :], 0.5, X[:, d, tsl(tt)], ALU.mult, ALU.add,
                                reads=[("ps", 2 * pd[f] + tt), ("X", d)], writes=[("X", d)])

        OFF_Q, OFF_K, OFF_V, OFF_GB, OFF_GC, OFF_XT, OFF_GA, OFF_GCV = 0, 2048, 4096, 6144, 8192, 10240, 12288, 14336

        def wcols(w, off, cp):
            return w[:, :, off + cp * 256: off + (cp + 1) * 256]

        def mixer_views():
            vs = []
            for cp in range(8):
                vs.append(wcols(wd["w_in"], OFF_GC, cp))
                vs.append(wcols(wd["w_in"], OFF_XT, cp))
            for cp in range(8):
                vs.append(wcols(wd["w_in"], OFF_K, cp))
            for cp in range(8):
                vs.append(wcols(wd["w_in"], OFF_V, cp))
            for cp in range(8):
                vs.append(wcols(wd["w_in"], OFF_GB, cp))
            for cp in range(8):
                vs.append(wcols(wd["w_conv_out"], 0, cp))
                vs.append(wcols(wd["w_in"], OFF_GCV, cp))
            for cp in range(8):
                vs.append(wcols(wd["w_in"], OFF_Q, cp))
            for cp in range(8):
                vs.append(wcols(wd["w_attn_out"], 0, cp))
                vs.append(wcols(wd["w_in"], OFF_GA, cp))
            for cp in range(8):
                vs.append(wcols(wd["w_out"], 0, cp))
            return vs

        def evac(i, out, in_, reads, writes):
            if i % 2 == 0:
                ACT(out, in_, AF.Copy, reads=reads, writes=writes)
            else:
                CP(out, in_, reads=reads, writes=writes)

        def mixer():
            scr_phase(tmpk)
            for cp in range(8):
                sa, sva = w_get()
                sb_, svb = w_get()
                pa = [next_pair(), next_pair()]
                pb = [next_pair(), next_pair()]
                for f in range(2):
                    mm_group(pa[f], proj_terms(sva, f, KC, H), reads=Hk + [("w", sa)])
                w_rel()
                for f in range(2):
                    mm_group(pb[f], proj_terms(svb, f, KC, H), reads=Hk + [("w", sb_)])
                    c = cp * 2 + f
                    for tt in range(2):
                        ti = next_tmp()
                        ACT(tmp4[ti], PS[2 * pa[f] + tt][:, :], AF.Copy, reads=[("ps", 2 * pa[f] + tt)],
                            writes=[("tmp", ti)])
                        TT(Rv[:, S1c + c, tsl(tt)], tmp4[ti], PS[2 * pb[f] + tt][:, :], ALU.mult,
                           reads=[("tmp", ti), ("ps", 2 * pb[f] + tt)], writes=[("R", S1c + c)])
                w_rel()
            CP(utail[:, :].rearrange("p (c t) -> p c t", t=2), Rv[:, S1c:S1c + 16, 1022:1024],
               reads=Rk(S1c, 16), writes=[("utail",)])
            DMA("sp", bounceU, utail[:, :], "ut", reads=[("utail",)], writes=[("bounceU",)])
            P.dma("pool", (lambda e: e.collective_compute("AllGather", ALU.bypass, replica_groups=RG,
                                                           ins=[bounceU], outs=[gathU])), "cc0",
                  reads=[("bounceU",)], writes=[("gathU",)], inc=1)
            if DEBUG_STOP == "AG0":
                return
            scr_phase([("kst", 0), ("kst", 1)])
            kst = [scr_bf(0, 1024), scr_bf(512, 1024)]
            kr = 0
            for cp in range(8):
                s, sv = w_get()
                pk = [next_pair(), next_pair()]
                for f in range(2):
                    mm_group(pk[f], proj_terms(sv, f, KC, H), reads=Hk + [("w", s)])
                    h = cp * 2 + f
                    ki = kr % 2
                    kr += 1
                    for tt in range(2):
                        evac(tt, kst[ki][:, tsl(tt)], PS[2 * pk[f] + tt][:, :], reads=[("ps", 2 * pk[f] + tt)],
                             writes=[("kst", ki)])
                    DMA("sp", bounceK[h // 8][(h % 8) * 128:(h % 8 + 1) * 128, :], kst[ki], f"kst{ki}",
                        reads=[("kst", ki)], writes=[("bounceK", h)])
                    if h % 8 == 7:
                        g = h // 8
                        P.dma("pool", (lambda e, g=g: e.collective_compute(
                            "AllGather", ALU.bypass, replica_groups=RG, ins=[bounceK[g]], outs=[gathK[g]])),
                            f"ccK{g}", reads=[("bounceK", hh) for hh in range(g * 8, g * 8 + 8)],
                            writes=[("gathK", g)], inc=1)
                w_rel()
            if DEBUG_STOP == "M2":
                return
            Vst = R[:, S2c * T:(S2c + 16) * T].rearrange("p (tc f) -> p tc f", f=2048)
            hb = 0
            for cp in range(8):
                s, sv = w_get()
                for tcn in range(8):
                    bank = hb % 8
                    hb += 1
                    pv = PS[bank][:, 0:256]

                    def emit(e, tcn=tcn, pv=pv, sv=sv):
                        ins = None
                        for k in range(KC):
                            ins = e.matmul(pv, H[:, k, tcn * 128:(tcn + 1) * 128], sv[:, k, 0:256],
                                           start=(k == 0), stop=(k == KC - 1))
                        return ins
                    P.op("pe", emit, reads=Hk + [("w", s)], writes=[("ps", bank)])
                    evac(tcn, Vst[:, tcn, cp * 256:(cp + 1) * 256], pv, reads=[("ps", bank)],
                         writes=[("R", S2c + 2 * tcn), ("R", S2c + 2 * tcn + 1)])
                w_rel()
            if DEBUG_STOP == "M3":
                return
            vdr = [bounceV[g].rearrange("(tc p two) c -> p tc two c", p=128, two=2) for g in range(2)]
            for tcn in range(8):
                DMA("sp", vdr[tcn // 4][:, tcn % 4, :, :], Vst[:, tcn, :].rearrange("p (two c) -> p two c", two=2),
                    "vst", reads=[("R", S2c + 2 * tcn), ("R", S2c + 2 * tcn + 1)], writes=[("bounceV", tcn)])
            vtot = P.dval["vst"]
            for tcn in range(8):
                P.state[("bounceV", tcn)][0] = {"vst": vtot}
                for k in (("R", S2c + 2 * tcn), ("R", S2c + 2 * tcn + 1)):
                    P.state[k][1]["vst"] = vtot
            if DEBUG_STOP == "M3d":
                return
            for g in range(2):
                P.dma("pool", (lambda e, g=g: e.collective_compute(
                    "AllGather", ALU.bypass, replica_groups=RG, ins=[bounceV[g]], outs=[gathV[g]])),
                    f"ccV{g}", reads=[("bounceV", t) for t in range(g * 4, g * 4 + 4)],
                    writes=[("gathV", g)], inc=1)
            if DEBUG_STOP == "AG1":
                return
            scr_phase([("cv",)])
            cv = scr_f32(0, 1024)
            DMA("sp", uhb[:, :], gathU[0:128, :], "uh", reads=[("gathU",)], writes=[("uhb",)])
            TS(uh[:, :], uhb[:, :], vecs[:, V_ODD:V_ODD + 1], None, ALU.mult, None, reads=[("uhb",), ("vecs",)],
               writes=[("uh",)])

            def cw(i, c):
                return vecs[:, V_CW + i * 16 + c: V_CW + i * 16 + c + 1]
            for cp in range(8):
                s, sv = w_get()
                pg = [next_pair(), next_pair()]
                for f in range(2):
                    mm_group(pg[f], proj_terms(sv, f, KC, H), reads=Hk + [("w", s)])
                w_rel()
                for f in range(2):
                    c = cp * 2 + f
                    uc = Rv[:, S1c + c, :]
                    rk = [("R", S1c + c)]
                    TS(cv, uc, cw(2, c), None, ALU.mult, None, reads=rk + [("vecs",)], writes=[("cv",)])
                    STT(cv[:, 1:1024], uc[:, 0:1023], cw(1, c), cv[:, 1:1024], ALU.mult, ALU.add,
                        reads=rk + [("cv",)], writes=[("cv",)])
                    STT(cv[:, 2:1024], uc[:, 0:1022], cw(0, c), cv[:, 2:1024], ALU.mult, ALU.add,
                        reads=rk + [("cv",)], writes=[("cv",)])
                    STT(cv[:, 0:1], uh[:, 2 * c + 1:2 * c + 2], cw(1, c), cv[:, 0:1], ALU.mult, ALU.add,
                        reads=[("uh",), ("cv",)], writes=[("cv",)])
                    STT(cv[:, 0:1], uh[:, 2 * c:2 * c + 1], cw(0, c), cv[:, 0:1], ALU.mult, ALU.add,
                        reads=[("uh",), ("cv",)], writes=[("cv",)])
                    STT(cv[:, 1:2], uh[:, 2 * c + 1:2 * c + 2], cw(0, c), cv[:, 1:2], ALU.mult, ALU.add,
                        reads=[("uh",), ("cv",)], writes=[("cv",)])
                    for tt in range(2):
                        TT(Rv[:, S1c + c, tsl(tt)], cv[:, tsl(tt)], PS[2 * pg[f] + tt][:, :], ALU.mult,
                           reads=[("cv",), ("ps", 2 * pg[f] + tt)], writes=[("R", S1c + c)])

            if DEBUG_STOP == "M4c":
                return
            def gated(src_c0, bcol, accumulate):
                scr_phase(tmpk)
                for cp in range(8):
                    sa, sva = w_get()
                    sb_, svb = w_get()
                    pa = [next_pair(), next_pair()]
                    pb = [next_pair(), next_pair()]
                    for f in range(2):
                        mm_group(pa[f], proj_terms(sva, f, KC, Rv, c0=src_c0), reads=Rk(src_c0, 16) + [("w", sa)])
                    w_rel()
                    for f in range(2):
                        mm_group(pb[f], proj_terms(svb, f, KC, H), reads=Hk + [("w", sb_)])
                        c = cp * 2 + f
                        for tt in range(2):
                            ti = next_tmp()
                            ACT(tmp4[ti], PS[2 * pb[f] + tt][:, :], AF.Sigmoid,
                                reads=[("ps", 2 * pb[f] + tt), ("vecs",)], writes=[("tmp", ti)],
                                bias=vecs[:, bcol + c:bcol + c + 1])
                            dst = Rv[:, S2c + c, tsl(tt)]
                            if not accumulate:
                                TT(dst, tmp4[ti], PS[2 * pa[f] + tt][:, :], ALU.mult,
                                   reads=[("tmp", ti), ("ps", 2 * pa[f] + tt)], writes=[("R", S2c + c)])
                            else:
                                TT(tmp4[ti], tmp4[ti], PS[2 * pa[f] + tt][:, :], ALU.mult,
                                   reads=[("tmp", ti), ("ps", 2 * pa[f] + tt)], writes=[("tmp", ti)])
                                TT(dst, tmp4[ti], dst, ALU.add, reads=[("tmp", ti), ("R", S2c + c)],
                                   writes=[("R", S2c + c)])
                    w_rel()
            gated(S1c, V_BG + 16, accumulate=False)
            if DEBUG_STOP == "M5":
                return
            for cp in range(8):
                s, sv = w_get()
                pq = [next_pair(), next_pair()]
                for f in range(2):
                    mm_group(pq[f], proj_terms(sv, f, KC, H), reads=Hk + [("w", s)])
                    h = cp * 2 + f
                    for tt in range(2):
                        evac(tt, Rv[:, S1c + h, tsl(tt)], PS[2 * pq[f] + tt][:, :], reads=[("ps", 2 * pq[f] + tt)],
                             writes=[("R", S1c + h)])
                w_rel()
            if DEBUG_STOP == "M4a":
                return
            attention()
            if DEBUG_STOP == "attn":
                return
            gated(S1c, V_BG, accumulate=True)
            for dp in range(8):
                s, sv = w_get()
                po = [next_pair(), next_pair()]
                for f in range(2):
                    mm_group(po[f], proj_terms(sv, f, KC, Rv, c0=S2c), reads=Rk(S2c, 16) + [("w", s)])
                    d = dp * 2 + f
                    for tt in range(2):
                        TT(X[:, d, tsl(tt)], PS[2 * po[f] + tt][:, :], X[:, d, tsl(tt)], ALU.add,
                           reads=[("ps", 2 * po[f] + tt), ("X", d)], writes=[("X", d)])
                w_rel()

        def attention():
            pT = [scr_bf(i * 128, 256) for i in range(6)]
            acc = [scr_f32(768 + i * 132, 132) for i in range(2)]
            obf = [scr_bf(1032 + i * 64, 128) for i in range(2)]
            rec = [scr_f32(1160 + i, 1) for i in range(2)]
            HB = [1168, 1376]
            gsc = [scr_f32(b_, 64) for b_ in HB]
            sel = [scr_f32(b_ + 64, 64) for b_ in HB]
            thr8 = [scr_f32(b_ + 128, 64) for b_ in HB]
            ksum = [scr_f32(b_ + 192, 8) for b_ in HB]
            kmb = [scr_bf(b_ + 200, 8) for b_ in HB]
            keys = ([("pT", i) for i in range(6)] + [("acc", i) for i in range(2)] + [("obf", i) for i in range(2)] +
                    [("rec", 0), ("rec", 1)] +
                    [(n_, b_) for n_ in ("gsc", "sel", "thr8", "ksum", "kmb") for b_ in range(2)])
            scr_phase(keys)
            for h in range(16):
                P.retarget([("R", S1c + h)], [("Q", h, i) for i in range(8)])
            KTb = [KT[:, :], W[:, 0:2048]]
            VVb = [VV[:, :, :], W[:, SLOT_E:SLOT_E + 16 * 129].rearrange("p (c d) -> p c d", d=129)]
            P.retarget([("KT", 0)], [("KT", 0, 0)])
            P.retarget([("KT", 1)], [("KT", 0, 1)])
            for g in range(4):
                P.retarget([("V", g)], [("V", 0, g)])
            P.retarget([("w", 0)], [("KT", 1, 0), ("KT", 1, 1)])
            P.retarget([("w", 1)], [("V", 1, g) for g in range(4)])
            psS = [PS[b][:, 0:256] for b in (0, 1, 2, 3, 4)]
            psO = [PS[b][:, 0:129] for b in (5, 6)]
            psG = PS[7][:, 0:64]
            psT = PS[7][:, 128:192].bitcast(BF16)
            psSk = [("ps", b) for b in (0, 1, 2, 3, 4)]
            psOk = [("ps", b) for b in (5, 6)]
            vview_g = [gathV[g][0:1024, :].rearrange("(tc p two) c -> p tc (two c)", p=128, two=2) for g in range(2)]
            vview_b = [bounceV[g].rearrange("(tc p two) c -> p tc (two c)", p=128, two=2) for g in range(2)]
            cnt = {"s": 0, "p": 0, "o": 0}
            mod = {"s": 5, "p": 6, "o": 2}

            def rr(k):
                v = cnt[k] % mod[k]
                cnt[k] += 1
                return v

            def load(h, b):
                hs = slice(h * 128, (h + 1) * 128)
                hg, hl = h // 8, slice((h % 8) * 128, (h % 8 + 1) * 128)
                DMA("sp", KTb[b][:, 0:1024], gathK[hg][hl, :], f"kt{2 * b}", reads=[("gathK", hg)],
                    writes=[("KT", b, 0)])
                DMA("sp", KTb[b][:, 1024:2048], bounceK[hg][hl, :], f"kt{2 * b + 1}", reads=[("bounceK", h)],
                    writes=[("KT", b, 1)])
                for g in range(2):
                    DMA("sp", VVb[b][:, 4 * g:4 * g + 4, 0:128], vview_g[g][:, :, hs], f"v{4 * b + g}",
                        reads=[("gathV", g)], writes=[("V", b, g)])
                    DMA("sp", VVb[b][:, 8 + 4 * g:12 + 4 * g, 0:128], vview_b[g][:, :, hs], f"v{4 * b + 2 + g}",
                        reads=[("bounceV", t) for t in range(4 * g, 4 * g + 4)], writes=[("V", b, 2 + g)])

            def setup(h, b):
                wh = vecs[:, V_W + h:V_W + h + 1]
                vk4 = [("V", b, g) for g in range(4)]
                TS(VVb[b][:, :, 0:128], VVb[b][:, :, 0:128], wh, None, ALU.mult, None, reads=vk4 + [("vecs",)],
                   writes=vk4)
                TS(VVb[b][:, :, 128:129], onec[:, :].rearrange("p (c o) -> p c o", o=1), wh, None, ALU.mult, None,
                   reads=vk4 + [("vecs",), ("onec",)], writes=vk4)
                QT = Rv[:, S1c + h, :]
                ktb = KTb[b]
                P.op("dve", (lambda e: e.tensor_reduce(out=ksum[b], in_=ktb.rearrange("p (b k) -> p b k", k=256),
                                                        axis=AX.X, op=ALU.add)),
                     reads=[("KT", b, 0), ("KT", b, 1)], writes=[("ksum", b)])
                TS(kmb[b], ksum[b], 1.0 / 256, None, ALU.mult, None, reads=[("ksum", b)], writes=[("kmb", b)])

                def emit_gate(e):
                    ins = None
                    for i in range(8):
                        ins = e.matmul(psG[:, i * 8:(i + 1) * 8], QT[:, i * 128:(i + 1) * 128], kmb[b], start=True,
                                       stop=True)
                    return ins
                P.op("pe", emit_gate, reads=[("Q", h, i) for i in range(8)] + [("kmb", b)], writes=[("ps", 7)])
                TT(gsc[b], psG, gmask[:, :], ALU.add, reads=[("ps", 7), ("gmask",)], writes=[("gsc", b)])
                for i in range(8):
                    P.op("dve", (lambda e, i=i: e.max(out=thr8[b][:, i * 8:(i + 1) * 8],
                                                      in_=gsc[b][:, i * 8:(i + 1) * 8])),
                         reads=[("gsc", b)], writes=[("thr8", b)])
                for i in range(8):
                    TS(sel[b][:, i * 8:(i + 1) * 8], gsc[b][:, i * 8:(i + 1) * 8], thr8[b][:, i * 8 + 2:i * 8 + 3],
                       None, ALU.is_ge, None, reads=[("gsc", b), ("thr8", b)], writes=[("sel", b)])
                STT(sel[b], gsc[b], -1.0e29, sel[b], ALU.is_gt, ALU.mult, reads=[("gsc", b), ("sel", b)],
                    writes=[("sel", b)])

            G = []
            for h in range(16):
                li = 0
                for m_ in range(4):
                    bl = [0, 1, 2, 3] + list(range(4, 4 + m_ + 1))
                    for bi, j in enumerate(bl):
                        for i in (2 * m_, 2 * m_ + 1):
                            diag = (j == 4 + i // 2)
                            kcs = [0] if (diag and i % 2 == 0) else [0, 1]
                            G.append(dict(h=h, li=li, i=i, bi=bi, j=j, diag=diag, kcs=kcs, last=(bi == len(bl) - 1)))
                            li += 1
                G[-1]["hlast"] = True

            def front(b):
                h, i, j, kcs = b["h"], b["i"], b["j"], b["kcs"]
                hb = h % 2
                QT = Rv[:, S1c + h, :]
                qs = slice(i * 128, (i + 1) * 128)
                si, pi = rr("s"), rr("p")
                b["pi"] = pi
                kkey = ("KT", hb, 0) if j < 4 else ("KT", hb, 1)
                ktb = KTb[hb]

                def emit_s(e):
                    ins = None
                    for kc in kcs:
                        vk = 2 * j + kc
                        ins = e.matmul(psS[si][:, kc * 128:(kc + 1) * 128], ktb[:, vk * 128:(vk + 1) * 128],
                                       QT[:, qs], start=True, stop=True)
                    return ins
                P.op("pe", emit_s, reads=[kkey, ("Q", h, i)], writes=[psSk[si]])
                for kc in kcs:
                    dc = 8 + i - (2 * j + kc)
                    ACT(pT[pi][:, kc * 128:(kc + 1) * 128], psS[si][:, kc * 128:(kc + 1) * 128], AF.Exp,
                        reads=[psSk[si]], writes=[("pT", pi)], scale=SCALE, bias=float(-SLOPES[h] * 128.0 * dc))
                if b["diag"]:
                    kd = i % 2
                    TT(pT[pi][:, kd * 128:(kd + 1) * 128], pT[pi][:, kd * 128:(kd + 1) * 128], tri[:, :],
                       ALU.mult, reads=[("pT", pi), ("tri",)], writes=[("pT", pi)])

            def back(b):
                h, i, j, kcs, pi = b["h"], b["i"], b["j"], b["kcs"], b["pi"]
                hb = h % 2
                QT = Rv[:, S1c + h, :]
                qs = slice(i * 128, (i + 1) * 128)
                ai = i % 2
                oi = rr("o")
                vkey = ("V", hb, j // 2)
                vvb = VVb[hb]

                def emit_pv(e):
                    ins = None
                    for n, kc in enumerate(kcs):
                        vk = 2 * j + kc
                        ins = e.matmul(psO[oi], pT[pi][:, kc * 128:(kc + 1) * 128], vvb[:, vk, 0:129],
                                       start=(n == 0), stop=(n == len(kcs) - 1))
                    return ins
                P.op("pe", emit_pv, reads=[("pT", pi), vkey], writes=[psOk[oi]])
                sc = sel[hb][:, i * 8 + j:i * 8 + j + 1]
                a129 = acc[ai][:, 0:129]
                if b["bi"] == 0:
                    TS(a129, psO[oi], sc, None, ALU.mult, None, reads=[psOk[oi], ("sel", hb)], writes=[("acc", ai)])
                elif b["diag"]:
                    TT(a129, psO[oi], a129, ALU.add, reads=[psOk[oi], ("acc", ai)], writes=[("acc", ai)])
                else:
                    STT(a129, psO[oi], sc, a129, ALU.mult, ALU.add, reads=[psOk[oi], ("sel", hb), ("acc", ai)],
                        writes=[("acc", ai)])
                if b["last"]:
                    P.op("dve", (lambda e: e.reciprocal(out=rec[ai], in_=acc[ai][:, 128:129])),
                         reads=[("acc", ai)], writes=[("rec", ai)])
                    TS(obf[ai], acc[ai][:, 0:128], rec[ai], None, ALU.mult, None,
                       reads=[("acc", ai), ("rec", ai)], writes=[("obf", ai)])

                    def fin():
                        P.op("pe", (lambda e: e.transpose(psT, obf[ai], ident[:, :])),
                             reads=[("obf", ai), ("ident",)], writes=[("ps", 7)])
                        ACT(QT[:, qs], psT, AF.Copy, reads=[("ps", 7)], writes=[("Q", h, i)])
                    return fin
                return None

            load(0, 0)
            setup(0, 0)
            load(1, 1)
            LA = 4
            pend = []
            ng = len(G)
            for idx in range(ng + LA):
                if idx < ng:
                    b = G[idx]
                    if b["li"] == 16 and b["h"] + 1 < 16:
                        setup(b["h"] + 1, (b["h"] + 1) % 2)
                    front(b)
                if idx >= LA:
                    b = G[idx - LA]
                    fin = back(b)
                    while pend and pend[0][0] <= idx:
                        pend.pop(0)[1]()
                    if fin is not None:
                        pend.append((idx + 2, fin))
                    if b.get("hlast") and b["h"] + 2 < 16:
                        load(b["h"] + 2, b["h"] % 2)
            for _, f_ in pend:
                f_()
            for h in range(16):
                P.retarget([("Q", h, i) for i in range(8)], [("R", S1c + h)])
            P.retarget([("KT", 1, 0), ("KT", 1, 1)], [("w", 0)])
            P.retarget([("V", 1, g) for g in range(4)], [("w", 1)])
            wstate["hold"] = False

        if "ffn1" in stages:
            wviews.extend(ffn_views("ffn1_w_gate", "ffn1_w_up", "ffn1_w_down"))
        if "mixer" in stages:
            wstate["hold_idx"] = len(wviews) + 64
            wstate["hold"] = True
            wviews.extend(mixer_views())
        if "ffn2" in stages:
            wviews.extend(ffn_views("ffn2_w_gate", "ffn2_w_up", "ffn2_w_down"))

        if "ffn1" in stages:
            rmsnorm(V_N1)
            ffn()
        if "mixer" in stages:
            rmsnorm(V_NM)
            mixer()
        if "ffn2" in stages:
            rmsnorm(V_N2)
            ffn()
        if "final" in stages:
            rmsnorm(V_NF, final=True)
        ov = outT.rearrange("(c p) t -> p c t", p=128)
        for c in range(KC):
            P.dma("sp", (lambda e, c=c: e.dma_start(out=ov[:, c, :], in_=X[:, c, :])), "out", reads=[("X", c)])
        out_total = P.dval["out"]
        P.streams["sp"].append(([("out", out_total)], None, None))
        assert DEBUG_STOP or wstate["consumed"] == len(wviews), (wstate, len(wviews))

        def run_stream(name, e):
            for waits, emit, sig in P.streams[name]:
                for s, v in waits:
                    e.wait_ge(sems[s], v)
                if emit is None:
                    continue
                ins = emit(e)
                ins.then_inc(sems[sig[0]], sig[1])

        with nc.Block() as block:
            @block.tensor
            def _(e):
                run_stream("pe", e)

            @block.scalar
            def _(e):
                run_stream("act", e)

            @block.vector
            def _(e):
                run_stream("dve", e)

            @block.gpsimd
            def _(e):
                run_stream("pool", e)

            @block.sync
            def _(e):
                run_stream("sp", e)
    return nc


def _host_consts():
    slopes = np.exp2(-8.0 * np.arange(1, 17, dtype=np.float64) / 16)
    p = np.arange(128, dtype=np.float64)[:, None]
    wtab = np.exp(slopes[None, :] * (p - 64.0)).astype(np.float32)
    k = np.arange(128)[:, None]
    q = np.arange(128)[None, :]
    tri = (k <= q).astype(np.float32).astype(ml_dtypes.bfloat16)
    ident = np.eye(128, dtype=np.float32).astype(ml_dtypes.bfloat16)
    return wtab, tri, ident


def _gmask(odd):
    g = np.full((128, 8, 8), -1.0e30, dtype=np.float32)
    for i in range(8):
        if odd:
            g[:, i, 0:4] = 0.0
        g[:, i, 4:4 + i // 2] = 0.0
    return np.ascontiguousarray(g.reshape(128, 64))


def _pc(v):
    v = np.asarray(v, np.float32)
    return v.reshape(-1, 128).T


def kernel(**inputs):
    x = np.asarray(inputs["x"], np.float32)
    wtab, tri, ident = _host_consts()
    common = {}
    for nm in ("ffn1_w_gate", "ffn1_w_up", "ffn1_w_down", "w_in", "w_attn_out", "w_conv_out", "w_out",
               "ffn2_w_gate", "ffn2_w_up", "ffn2_w_down"):
        common[nm] = np.ascontiguousarray(np.asarray(inputs[nm], np.float32)[0])
    vec = np.zeros((128, V_TOT), np.float32)
    vec[:, V_N1:V_N1 + 16] = _pc(inputs["ffn1_norm"][0])
    vec[:, V_NM:V_NM + 16] = _pc(inputs["mix_norm"][0])
    vec[:, V_N2:V_N2 + 16] = _pc(inputs["ffn2_norm"][0])
    vec[:, V_NF:V_NF + 16] = _pc(inputs["final_norm"])
    vec[:, V_BG:V_BG + 32] = _pc(inputs["b_gate"][0])
    cwv = np.asarray(inputs["conv_w"], np.float32)[0]
    for i in range(3):
        vec[:, V_CW + 16 * i:V_CW + 16 * (i + 1)] = _pc(cwv[i])
    vec[:, V_W:V_W + 16] = wtab
    common.update(tri=tri, ident=ident)
    in_maps = []
    for c in range(8):
        b, half = c // 2, c % 2
        m = dict(common)
        m["xT"] = np.ascontiguousarray(x[b, half * T:(half + 1) * T, :].T)
        v = vec.copy()
        v[:, V_ODD] = float(half)
        m["vecs"] = v
        m["gmask"] = _gmask(half == 1)
        in_maps.append(m)
    nc = build_program()
    res = run_bass_kernel_spmd(nc, in_maps, core_ids=list(range(8)))
    out = np.empty((4, 2048, D), np.float32)
    for c in range(8):
        b, half = c // 2, c % 2
        out[b, half * T:(half + 1) * T, :] = np.asarray(res.results[c]["outT"], np.float32).T
    return out
```
